# Optimizing a Trainium2 kernel written in Bass

```python
import jax
import jax.numpy as jnp
from jax import lax
import numpy as np

D_MODEL = 1024
BATCH = 4
SEQ = 8192
DEPTH = 4

HEAD_DIM = 64
MIX_HEADS = 12
MIX_WIDTH = MIX_HEADS * HEAD_DIM
MEM_HEADS = 4
MEM_WIDTH = MEM_HEADS * HEAD_DIM
MEM_LEN = 256
CONV_WIDTH = 3
ROPE_DIM = HEAD_DIM // 4
ROPE_THETA = 500000.0
MOBA_BLOCK = 256
MOBA_TOPK = 3
MOBA_Q_CHUNK = 16
D_FF = 2816
N_EXPERTS = 8
TOP_K = 2
LN_EPS = 1e-5
N_A_LAYERS = DEPTH // 2
N_B_LAYERS = DEPTH - N_A_LAYERS
N_DENSE = (DEPTH + 1) // 2
N_MOE = DEPTH // 2
DEEPNORM_ALPHA = float((2 * DEPTH) ** 0.25)
DEEPNORM_BETA = float((8 * DEPTH) ** -0.25)
ATTN_SCALE = HEAD_DIM ** -0.5

kernel_name = 'hybrid_shortconv_moba_yoco_deepnorm_moe'


def layer_norm(x, g, b):
    xf = x.astype(jnp.float32)
    mu = jnp.mean(xf, axis=-1, keepdims=True)
    var = jnp.mean(jnp.square(xf - mu), axis=-1, keepdims=True)
    y = (xf - mu) * lax.rsqrt(var + LN_EPS)
    return (y * g + b).astype(x.dtype)


def rope_tables(positions):
    inv_freq = ROPE_THETA ** (-jnp.arange(0, ROPE_DIM, 2, dtype=jnp.float32) / ROPE_DIM)
    angles = positions.astype(jnp.float32)[..., None] * inv_freq
    return jnp.cos(angles)[:, :, None, :], jnp.sin(angles)[:, :, None, :]


def apply_partial_rope(t, cos, sin):
    half = ROPE_DIM // 2
    t1 = t[..., :half].astype(jnp.float32)
    t2 = t[..., half:ROPE_DIM].astype(jnp.float32)
    rot = jnp.concatenate([t1 * cos - t2 * sin, t2 * cos + t1 * sin], axis=-1).astype(t.dtype)
    return jnp.concatenate([rot, t[..., ROPE_DIM:]], axis=-1)


def short_conv_mixer(u_b, u_c, u_h, conv_w):
    z = u_c * u_h
    z = lax.conv_general_dilated(
        z, conv_w[:, None, :], window_strides=(1,), padding=[(CONV_WIDTH - 1, 0)],
        dimension_numbers=('NWC', 'WIO', 'NWC'), feature_group_count=MIX_WIDTH)
    return u_b * z


def memory_cross_attention(q, k, v):
    logits = jnp.einsum('bshd,bmhd->bhsm', q, k).astype(jnp.float32) * ATTN_SCALE
    probs = jax.nn.softmax(logits, axis=-1).astype(v.dtype)
    out = jnp.einsum('bhsm,bmhd->bshd', probs, v)
    return out.reshape(q.shape[0], q.shape[1], -1)


def shared_key_value(h, w_kv, cos, sin):
    bsz, seq, _ = h.shape
    kv = jnp.einsum('bsd,de->bse', h, w_kv)
    k = apply_partial_rope(kv[..., :MIX_WIDTH].reshape(bsz, seq, MIX_HEADS, HEAD_DIM), cos, sin)
    v = kv[..., MIX_WIDTH:].reshape(bsz, seq, MIX_HEADS, HEAD_DIM)
    pad = (-seq) % MOBA_BLOCK
    k = jnp.pad(k, ((0, 0), (0, pad), (0, 0), (0, 0)))
    v = jnp.pad(v, ((0, 0), (0, pad), (0, 0), (0, 0)))
    n_blocks = (seq + pad) // MOBA_BLOCK
    k_blocks = k.reshape(bsz, n_blocks, MOBA_BLOCK, MIX_HEADS, HEAD_DIM).transpose(0, 3, 1, 2, 4)
    v_blocks = v.reshape(bsz, n_blocks, MOBA_BLOCK, MIX_HEADS, HEAD_DIM).transpose(0, 3, 1, 2, 4)
    k_means = jnp.mean(k_blocks.astype(jnp.float32), axis=3).astype(k.dtype)
    return k_blocks, v_blocks, k_means


def moba_attention(q, k_blocks, v_blocks, k_means):
    bsz, seq, n_heads, head_dim = q.shape
    n_blocks = k_blocks.shape[2]
    topk = min(MOBA_TOPK, n_blocks)
    n_sel = topk * MOBA_BLOCK
    n_chunks = seq // MOBA_Q_CHUNK
    q_chunks = q.reshape(bsz, n_chunks, MOBA_Q_CHUNK, n_heads, head_dim).transpose(1, 0, 3, 2, 4)
    b_idx = jnp.arange(bsz)[:, None, None, None]
    h_idx = jnp.arange(n_heads)[None, :, None, None]
    block_ids = jnp.arange(n_blocks)

    def one_chunk(args):
        c, qc = args
        start = c * MOBA_Q_CHUNK
        own = start // MOBA_BLOCK
        q_pos = start + jnp.arange(MOBA_Q_CHUNK)
        gate = jnp.einsum('bhqd,bhnd->bhqn', qc, k_means).astype(jnp.float32)
        gate = jnp.where(block_ids < own, gate, -jnp.inf)
        _, sel = lax.top_k(gate, topk)
        sel_valid = sel < own
        k_sel = k_blocks[b_idx, h_idx, sel]
        v_sel = v_blocks[b_idx, h_idx, sel]
        k_own = lax.dynamic_index_in_dim(k_blocks, own, axis=2, keepdims=False)
        v_own = lax.dynamic_index_in_dim(v_blocks, own, axis=2, keepdims=False)
        s_sel = jnp.einsum('bhqd,bhqkjd->bhqkj', qc, k_sel).astype(jnp.float32) * ATTN_SCALE
        s_sel = jnp.where(sel_valid[..., None], s_sel, -jnp.inf)
        s_own = jnp.einsum('bhqd,bhjd->bhqj', qc, k_own).astype(jnp.float32) * ATTN_SCALE
        k_pos = own * MOBA_BLOCK + jnp.arange(MOBA_BLOCK)
        s_own = jnp.where(k_pos[None, :] <= q_pos[:, None], s_own, -jnp.inf)
        scores = jnp.concatenate([s_sel.reshape(bsz, n_heads, MOBA_Q_CHUNK, n_sel), s_own], axis=-1)
        probs = jax.nn.softmax(scores, axis=-1).astype(qc.dtype)
        p_sel = probs[..., :n_sel].reshape(bsz, n_heads, MOBA_Q_CHUNK, topk, MOBA_BLOCK)
        p_own = probs[..., n_sel:]
        return (jnp.einsum('bhqkj,bhqkjd->bhqd', p_sel, v_sel)
                + jnp.einsum('bhqj,bhjd->bhqd', p_own, v_own))

    out = lax.map(one_chunk, (jnp.arange(n_chunks), q_chunks))
    return out.transpose(1, 0, 3, 2, 4).reshape(bsz, seq, n_heads * head_dim)


def swiglu(x, w_gu, w_down):
    gu = jnp.einsum('bsd,df->bsf', x, w_gu)
    g, u = gu[..., :D_FF], gu[..., D_FF:]
    return jnp.einsum('bsf,fd->bsd', jax.nn.silu(g) * u, w_down)


def moe_swiglu(x, w_router, w_gu, w_down):
    logits = jnp.einsum('bsd,de->bse', x, w_router).astype(jnp.float32)
    top_logits, top_idx = lax.top_k(logits, TOP_K)
    top_gates = jax.nn.softmax(top_logits, axis=-1)
    gates = jnp.sum(jax.nn.one_hot(top_idx, N_EXPERTS, dtype=jnp.float32) * top_gates[..., None], axis=-2)
    gates = gates.astype(x.dtype)
    out = jnp.zeros_like(x)
    for e in range(N_EXPERTS):
        out = out + gates[..., e:e + 1] * swiglu(x, w_gu[e], w_down[e])
    return out


def setup_inputs(seed: int = 0) -> dict:
    key = jax.random.key(seed)
    ks = jax.random.split(key, 20)
    f32 = jnp.float32

    def nrm(k, shape, scale):
        return jax.random.normal(k, shape, f32) * scale

    x = nrm(ks[0], (BATCH, SEQ, D_MODEL), 1.0)
    mem = nrm(ks[1], (BATCH, MEM_LEN, D_MODEL), 1.0)
    offset = jax.random.randint(ks[2], (BATCH, 1), 0, 1024, dtype=jnp.int32)
    positions = offset + jnp.arange(SEQ, dtype=jnp.int32)[None, :]
    w_in_a = nrm(ks[3], (N_A_LAYERS, D_MODEL, 3 * MIX_WIDTH + MEM_WIDTH), D_MODEL ** -0.5)
    conv_a = nrm(ks[4], (N_A_LAYERS, CONV_WIDTH, MIX_WIDTH), CONV_WIDTH ** -0.5)
    w_q_b = nrm(ks[5], (N_B_LAYERS, D_MODEL, MIX_WIDTH + MEM_WIDTH), D_MODEL ** -0.5)
    w_kv_shared = nrm(ks[6], (D_MODEL, 2 * MIX_WIDTH), D_MODEL ** -0.5)
    w_mem_kv = nrm(ks[7], (DEPTH, D_MODEL, 2 * MEM_WIDTH), D_MODEL ** -0.5)
    w_o = nrm(ks[8], (DEPTH, MIX_WIDTH + MEM_WIDTH, D_MODEL), (MIX_WIDTH + MEM_WIDTH) ** -0.5 * DEEPNORM_BETA)
    ln1_g = 1.0 + nrm(ks[9], (DEPTH, D_MODEL), 0.02)
    ln1_b = nrm(ks[10], (DEPTH, D_MODEL), 0.02)
    ln2_g = 1.0 + nrm(ks[11], (DEPTH, D_MODEL), 0.02)
    ln2_b = nrm(ks[12], (DEPTH, D_MODEL), 0.02)
    w_gu_dense = nrm(ks[13], (N_DENSE, D_MODEL, 2 * D_FF), D_MODEL ** -0.5)
    w_down_dense = nrm(ks[14], (N_DENSE, D_FF, D_MODEL), D_FF ** -0.5 * DEEPNORM_BETA)
    w_router = nrm(ks[15], (N_MOE, D_MODEL, N_EXPERTS), D_MODEL ** -0.5)
    w_gu_moe = nrm(ks[16], (N_MOE, N_EXPERTS, D_MODEL, 2 * D_FF), D_MODEL ** -0.5)
    w_down_moe = nrm(ks[17], (N_MOE, N_EXPERTS, D_FF, D_MODEL), D_FF ** -0.5 * DEEPNORM_BETA)
    return {'x': x, 'mem': mem, 'positions': positions, 'w_in_a': w_in_a, 'conv_a': conv_a,
            'w_q_b': w_q_b, 'w_kv_shared': w_kv_shared, 'w_mem_kv': w_mem_kv, 'w_o': w_o,
            'ln1_g': ln1_g, 'ln1_b': ln1_b, 'ln2_g': ln2_g, 'ln2_b': ln2_b,
            'w_gu_dense': w_gu_dense, 'w_down_dense': w_down_dense, 'w_router': w_router,
            'w_gu_moe': w_gu_moe, 'w_down_moe': w_down_moe}


def reference(x, mem, positions, w_in_a, conv_a, w_q_b, w_kv_shared, w_mem_kv, w_o,
              ln1_g, ln1_b, ln2_g, ln2_b, w_gu_dense, w_down_dense, w_router,
              w_gu_moe, w_down_moe):
    bsz, seq, _ = x.shape
    cos, sin = rope_tables(positions)
    shared = None
    for layer in range(DEPTH):
        mem_kv = jnp.einsum('bmd,de->bme', mem, w_mem_kv[layer])
        mem_k = mem_kv[..., :MEM_WIDTH].reshape(bsz, -1, MEM_HEADS, HEAD_DIM)
        mem_v = mem_kv[..., MEM_WIDTH:].reshape(bsz, -1, MEM_HEADS, HEAD_DIM)
        if layer < N_A_LAYERS:
            proj = jnp.einsum('bsd,de->bse', x, w_in_a[layer])
            u_b = proj[..., :MIX_WIDTH]
            u_c = proj[..., MIX_WIDTH:2 * MIX_WIDTH]
            u_h = proj[..., 2 * MIX_WIDTH:3 * MIX_WIDTH]
            q_mem = proj[..., 3 * MIX_WIDTH:]
            main = short_conv_mixer(u_b, u_c, u_h, conv_a[layer])
        else:
            proj = jnp.einsum('bsd,de->bse', x, w_q_b[layer - N_A_LAYERS])
            q = apply_partial_rope(proj[..., :MIX_WIDTH].reshape(bsz, seq, MIX_HEADS, HEAD_DIM), cos, sin)
            q_mem = proj[..., MIX_WIDTH:]
            k_blocks, v_blocks, k_means = shared
            main = moba_attention(q, k_blocks, v_blocks, k_means)
        mem_out = memory_cross_attention(q_mem.reshape(bsz, seq, MEM_HEADS, HEAD_DIM), mem_k, mem_v)
        mix = jnp.einsum('bse,ed->bsd', jnp.concatenate([main, mem_out], axis=-1), w_o[layer])
        x = layer_norm(DEEPNORM_ALPHA * x + mix, ln1_g[layer], ln1_b[layer])
        if layer % 2 == 0:
            ffn = swiglu(x, w_gu_dense[layer // 2], w_down_dense[layer // 2])
        else:
            ffn = moe_swiglu(x, w_router[layer // 2], w_gu_moe[layer // 2], w_down_moe[layer // 2])
        x = layer_norm(DEEPNORM_ALPHA * x + ffn, ln2_g[layer], ln2_b[layer])
        if layer == N_A_LAYERS - 1:
            shared = shared_key_value(x, w_kv_shared, cos, sin)
    return x
```

```python
from contextlib import ExitStack
import numpy as np
import ml_dtypes
import concourse.bass as bass
import concourse.mybir as mybir
from concourse.bass_utils import run_bass_kernel_spmd

F32 = mybir.dt.float32
BF16 = mybir.dt.bfloat16
I32 = mybir.dt.int32
ALU = mybir.AluOpType
AF = mybir.ActivationFunctionType

D = 1024
SEQ = 8192
NBATCH = 4
DEPTH = 4
MIXW = 768
MEMW = 256
MEML = 256
DFF = 2816
NEXP = 8
NFC = DFF // 128
ALPHA = float((2 * DEPTH) ** 0.25)
EPS = 1e-5
BLK = 256
NBLK = SEQ // BLK
NLOC = 16
NSUP = 4
NT = 1040
MAINC = 1024
SUBS = [(s * 128, 128) for s in range(8)] + [(1024, 16)]
CG_ALL = [(0, 512), (512, 512), (1024, 16)]
CG_MAIN = [(0, 512), (512, 512)]
NEGBIG = -240000.0
NDMA_SEM = 6
RING_SLOTS = 7


def glob_block(i, h):
    m, r = divmod(i, 2)
    if r == 0:
        return 4 * m + (0 if h == 0 else 1)
    return 4 * m + (3 if h == 0 else 2)


GLOB_INV = {glob_block(i, h): (h, i) for h in range(2) for i in range(NLOC)}


class Buf:
    __slots__ = ("name", "writer", "readers", "excl")

    def __init__(self, name="", excl=False):
        self.name = name
        self.writer = None
        self.readers = []
        self.excl = excl


class Op:
    __slots__ = ("eng", "fn", "deps", "signal", "ticket", "is_dma", "dsem", "dval")

    def __init__(self, eng, fn, is_dma=False):
        self.eng = eng
        self.fn = fn
        self.deps = []
        self.signal = False
        self.ticket = None
        self.is_dma = is_dma
        self.dsem = None
        self.dval = None


class Prog:
    ENGS = ("pe", "act", "dve", "pool", "sp")

    def __init__(self, nc):
        self.nc = nc
        self.q = {e: [] for e in self.ENGS}
        self.ndma = {e: 0 for e in self.ENGS}

    def op(self, eng, fn, reads=(), writes=(), deps=(), dma=False):
        o = Op(eng, fn, is_dma=dma)
        ds = set()
        for d in deps:
            if d is not None:
                ds.add(d)
        for b in reads:
            if b.writer is not None:
                ds.add(b.writer)
            if b.excl:
                for r in b.readers:
                    if r.eng != eng:
                        ds.add(r)
        for b in writes:
            if b.writer is not None:
                ds.add(b.writer)
            for r in b.readers:
                ds.add(r)
        for d in ds:
            if d is o:
                continue
            if (not d.is_dma) and d.eng == "pe" and eng == "pe" and not dma:
                continue
            d.signal = True
            o.deps.append(d)
        for b in reads:
            b.readers.append(o)
        for b in writes:
            b.writer = o
            b.readers = []
        if dma:
            o.signal = True
            k = self.ndma[eng]
            self.ndma[eng] += 1
            o.dsem = k % NDMA_SEM
            o.dval = 16 * (k // NDMA_SEM + 1)
        self.q[eng].append(o)
        return o

    def dma(self, eng, out, in_, reads=(), writes=(), deps=()):
        return self.op(eng, lambda e: e.dma_start(out=out, in_=in_), reads=reads, writes=writes,
                       deps=deps, dma=True)

    def emit(self, final_waits=()):
        nc = self.nc
        for e in self.ENGS:
            t = 0
            for o in self.q[e]:
                if o.signal and not o.is_dma:
                    t += 1
                    o.ticket = t
        with ExitStack() as st:
            sems = {e: st.enter_context(nc.semaphore("s_" + e)) for e in self.ENGS}
            dsems = {e: [st.enter_context(nc.semaphore("d_%s%d" % (e, i))) for i in range(NDMA_SEM)]
                     for e in ("sp", "pool")}
            block = st.enter_context(nc.Block())
            prog = self

            def run(ename, eh):
                waited = {}
                for o in prog.q[ename]:
                    for d in o.deps:
                        if d.is_dma:
                            key = ("d", d.eng, d.dsem)
                            val = d.dval
                            sem = dsems[d.eng][d.dsem]
                        else:
                            key = ("e", d.eng)
                            val = d.ticket
                            sem = sems[d.eng]
                        if waited.get(key, 0) < val:
                            eh.wait_ge(sem, val)
                            waited[key] = val
                    if o.is_dma:
                        if o.dval > 16:
                            key = ("d", ename, o.dsem)
                            if waited.get(key, 0) < o.dval - 16:
                                eh.wait_ge(dsems[ename][o.dsem], o.dval - 16)
                                waited[key] = o.dval - 16
                        inst = o.fn(eh)
                        inst.then_inc(dsems[ename][o.dsem], 16)
                    else:
                        inst = o.fn(eh)
                        if o.signal:
                            inst.then_inc(sems[ename], 1)
                if ename == "sp":
                    for d in final_waits:
                        if d.is_dma:
                            eh.wait_ge(dsems[d.eng][d.dsem], d.dval)
                        else:
                            eh.wait_ge(sems[d.eng], d.ticket)

            @block.tensor
            def _(e):
                run("pe", e)

            @block.scalar
            def _(e):
                run("act", e)

            @block.vector
            def _(e):
                run("dve", e)

            @block.gpsimd
            def _(e):
                run("pool", e)

            @block.sync
            def _(e):
                run("sp", e)


class Rot:
    def __init__(self, tiles, excl=False):
        self.tiles = tiles
        self.bufs = [Buf(excl=excl) for _ in tiles]
        self.i = 0

    def next(self):
        k = self.i % len(self.tiles)
        self.i += 1
        return self.tiles[k], self.bufs[k]


class K:
    def __init__(self, nc, st, nsup):
        self.nc = nc
        self.st = st
        self.P = Prog(nc)
        self.nsup = nsup
        self.outs = []
        P = self.P
        sb = self.sb
        self.ident = sb("ident", [128, 128], BF16)
        self.b_ident = Buf()
        self.ones = sb("ones", [128, 64], BF16)
        self.b_ones = Buf()
        self.mm = Rot([st.enter_context(nc.psum_tensor("mm%d" % i, [128, 512], F32)) for i in range(4)], excl=True)
        self.po = Rot([st.enter_context(nc.psum_tensor("po%d" % i, [128, 512], F32)) for i in range(2)], excl=True)
        self.tp = Rot([st.enter_context(nc.psum_tensor("tp%d" % i, [128, 1024], BF16)) for i in range(2)], excl=True)
        self.ring = Rot([sb("ring%d" % i, [128, 4096], BF16) for i in range(RING_SLOTS)])
        self.xres = [sb("xres%d" % s, [128, D], F32) for s in range(9)]
        self.b_xres = [Buf() for _ in range(9)]
        self.xT = sb("xT", [128, 8, NT], BF16)
        self.b_xT = [Buf() for _ in range(9)]
        self.big = sb("big", [128, NFC * NT], BF16)
        self.xbf = Rot([sb("xbf%d" % i, [128, D], BF16) for i in range(2)])
        self.lnp = [sb("lnp%d" % i, [128, D], F32) for i in range(4)]
        self.b_lnp = [Buf() for _ in range(4)]
        self.stats = Rot([sb("stats%d" % i, [128, 32], F32) for i in range(4)])

    def sb(self, name, shape, dt):
        return self.st.enter_context(self.nc.sbuf_tensor("sb_" + name, shape, dt))

    def wload(self, src, a, b):
        t, buf = self.ring.next()
        assert a * b <= 4096
        dst = t[:, 0:a * b].rearrange("p (a b) -> p a b", a=a, b=b)
        self.P.dma("pool", dst, src, writes=[buf])
        return dst, buf

    def make_xT(self, s):
        P = self.P
        c0, nt = SUBS[s]
        xb, b_xb = self.xbf.next()
        src = self.xres[s]
        P.op("act", lambda e: e.activation(out=xb[0:nt, :], in_=src[0:nt, :], func=AF.Copy),
             reads=[self.b_xres[s]], writes=[b_xb])
        for half in range(2):
            tp, b_tp = self.tp.next()
            for j in range(4):
                kc = half * 4 + j
                P.op("pe", lambda e, kc=kc, j=j, tp=tp: e.transpose(
                    out=tp[:, j * 128:j * 128 + nt], in_=xb[0:nt, kc * 128:(kc + 1) * 128],
                    identity=self.ident[0:nt, 0:nt]),
                    reads=[b_xb, self.b_ident], writes=[b_tp])
            srcv = tp[:, 0:512].rearrange("p (a b) -> p a b", a=4, b=128)[:, :, 0:nt]
            dstv = self.xT[:, half * 4:(half + 1) * 4, c0:c0 + nt]
            if half == 0:
                P.op("dve", lambda e, srcv=srcv, dstv=dstv: e.tensor_copy(out=dstv, in_=srcv),
                     reads=[b_tp], writes=[self.b_xT[s]])
            else:
                P.op("act", lambda e, srcv=srcv, dstv=dstv: e.activation(out=dstv, in_=srcv, func=AF.Copy),
                     reads=[b_tp], writes=[self.b_xT[s]])

    def xT_bufs(self, c0, n):
        return [self.b_xT[s] for s, (o, k) in enumerate(SUBS) if o < c0 + n and o + k > c0]

    def load_ln(self, g_ap, b_ap, which):
        P = self.P
        for k, ap in enumerate((g_ap, b_ap)):
            i = which * 2 + k
            P.dma("sp", self.lnp[i][:], ap.broadcast_to([128, D]), writes=[self.b_lnp[i]])

    def layer_norm(self, s, which):
        P = self.P
        c0, nt = SUBS[s]
        x = self.xres[s]
        bx = self.b_xres[s]
        stt, b_st = self.stats.next()
        for hh in range(2):
            P.op("dve", lambda e, hh=hh: e.bn_stats(out=stt[0:nt, hh * 6:(hh + 1) * 6],
                                                     in_=x[0:nt, hh * 512:(hh + 1) * 512]),
                 reads=[bx], writes=[b_st])
        P.op("dve", lambda e: e.bn_aggr(out=stt[0:nt, 12:14],
                                        in_=stt[0:nt, 0:12].rearrange("p (a b) -> p a b", a=2, b=6)),
             reads=[b_st], writes=[b_st])
        P.op("dve", lambda e: e.tensor_scalar(out=stt[0:nt, 14:15], in0=stt[0:nt, 13:14], scalar1=EPS,
                                              scalar2=None, op0=ALU.add), reads=[b_st], writes=[b_st])
        P.op("act", lambda e: e.activation(out=stt[0:nt, 15:16], in_=stt[0:nt, 14:15], func=AF.Sqrt),
             reads=[b_st], writes=[b_st])
        P.op("dve", lambda e: e.reciprocal(out=stt[0:nt, 16:17], in_=stt[0:nt, 15:16]),
             reads=[b_st], writes=[b_st])
        P.op("dve", lambda e: e.tensor_scalar(out=x[0:nt, :], in0=x[0:nt, :], scalar1=stt[0:nt, 12:13],
                                              scalar2=stt[0:nt, 16:17], op0=ALU.subtract, op1=ALU.mult),
             reads=[bx, b_st], writes=[bx])
        g = self.lnp[which * 2]
        b = self.lnp[which * 2 + 1]
        P.op("dve", lambda e: e.tensor_tensor(out=x[0:nt, :], in0=x[0:nt, :], in1=g[0:nt, :], op=ALU.mult),
             reads=[bx, self.b_lnp[which * 2]], writes=[bx])
        P.op("pool", lambda e: e.tensor_tensor(out=x[0:nt, :], in0=x[0:nt, :], in1=b[0:nt, :], op=ALU.add),
             reads=[bx, self.b_lnp[which * 2 + 1]], writes=[bx])

    def ffn(self, w_gu, w_down, subs, cgs, w_router=None):
        P = self.P
        nexp = len(w_gu)
        gates = None
        if w_router is not None:
            gates, b_gates = self.gates, self.b_gates
            wr, b_wr = self.wload(w_router.rearrange("(kc p) e -> p kc e", p=128), 8, NEXP)
            for s in subs:
                c0, nt = SUBS[s]
                ps, b_ps = self.po.next()
                for kc in range(8):
                    P.op("pe", lambda e, kc=kc, ps=ps, c0=c0, nt=nt: e.matmul(
                        ps[0:nt, 0:NEXP], lhsT=self.xT[:, kc, c0:c0 + nt], rhs=wr[:, kc, :],
                        start=(kc == 0), stop=(kc == 7)), reads=[self.b_xT[s], b_wr], writes=[b_ps])
                g = gates[:, s, :]
                P.op("dve", lambda e, g=g, ps=ps, nt=nt: e.tensor_copy(out=g[0:nt, 0:8], in_=ps[0:nt, 0:8]),
                     reads=[b_ps], writes=[b_gates[s]])
                P.op("dve", lambda e, g=g, nt=nt: e.max(out=g[0:nt, 8:16], in_=g[0:nt, 0:8]),
                     reads=[b_gates[s]], writes=[b_gates[s]])
                P.op("dve", lambda e, g=g, nt=nt: e.tensor_scalar(
                    out=g[0:nt, 16:24], in0=g[0:nt, 0:8], scalar1=g[0:nt, 9:10], scalar2=None, op0=ALU.is_ge),
                    reads=[b_gates[s]], writes=[b_gates[s]])
                P.op("dve", lambda e, g=g, nt=nt: e.tensor_scalar(
                    out=g[0:nt, 32:33], in0=g[0:nt, 8:9], scalar1=-1.0, scalar2=None, op0=ALU.mult),
                    reads=[b_gates[s]], writes=[b_gates[s]])
                P.op("act", lambda e, g=g, nt=nt: e.activation(
                    out=g[0:nt, 24:32], in_=g[0:nt, 0:8], func=AF.Exp, bias=g[0:nt, 32:33], scale=1.0),
                    reads=[b_gates[s]], writes=[b_gates[s]])
                P.op("dve", lambda e, g=g, nt=nt: e.tensor_tensor(
                    out=g[0:nt, 24:32], in0=g[0:nt, 24:32], in1=g[0:nt, 16:24], op=ALU.mult),
                    reads=[b_gates[s]], writes=[b_gates[s]])
                P.op("dve", lambda e, g=g, nt=nt: e.tensor_reduce(
                    out=g[0:nt, 33:34], in_=g[0:nt, 24:32], axis=mybir.AxisListType.X, op=ALU.add),
                    reads=[b_gates[s]], writes=[b_gates[s]])
                P.op("dve", lambda e, g=g, nt=nt: e.reciprocal(out=g[0:nt, 34:35], in_=g[0:nt, 33:34]),
                     reads=[b_gates[s]], writes=[b_gates[s]])
                P.op("dve", lambda e, g=g, nt=nt: e.tensor_scalar(
                    out=g[0:nt, 16:24], in0=g[0:nt, 24:32], scalar1=g[0:nt, 34:35], scalar2=None, op0=ALU.mult),
                    reads=[b_gates[s]], writes=[b_gates[s]])
        for s in subs:
            c0, nt = SUBS[s]
            x = self.xres[s]
            P.op("act", lambda e, x=x, nt=nt: e.activation(out=x[0:nt, :], in_=x[0:nt, :], func=AF.Copy, scale=ALPHA),
                 reads=[self.b_xres[s]], writes=[self.b_xres[s]])
        act = self.big[:, :].rearrange("p (a b) -> p a b", a=NFC, b=NT)
        b_act = self.b_big
        fgroups = [(0, 4), (4, 4), (8, 4), (12, 4), (16, 4), (20, 2)]
        for ex in range(nexp):
            gu = w_gu[ex].rearrange("(kc p) f -> p kc f", p=128)
            for (f0, nf) in fgroups:
                wg, b_wg = self.wload(gu[:, :, f0 * 128:(f0 + nf) * 128], 8, nf * 128)
                wu, b_wu = self.wload(gu[:, :, DFF + f0 * 128:DFF + (f0 + nf) * 128], 8, nf * 128)
                for j in range(nf):
                    fc = f0 + j
                    for (c0, n) in cgs:
                        pg, b_pg = self.mm.next()
                        pu, b_pu = self.mm.next()
                        xb = self.xT_bufs(c0, n)
                        for kc in range(8):
                            P.op("pe", lambda e, kc=kc, pg=pg, j=j, c0=c0, n=n, wg=wg: e.matmul(
                                pg[:, 0:n], lhsT=wg[:, kc, j * 128:(j + 1) * 128], rhs=self.xT[:, kc, c0:c0 + n],
                                start=(kc == 0), stop=(kc == 7)), reads=xb + [b_wg], writes=[b_pg])
                        for kc in range(8):
                            P.op("pe", lambda e, kc=kc, pu=pu, j=j, c0=c0, n=n, wu=wu: e.matmul(
                                pu[:, 0:n], lhsT=wu[:, kc, j * 128:(j + 1) * 128], rhs=self.xT[:, kc, c0:c0 + n],
                                start=(kc == 0), stop=(kc == 7)), reads=xb + [b_wu], writes=[b_pu])
                        sg, b_sg = self.sgt.next()
                        P.op("act", lambda e, sg=sg, pg=pg, n=n: e.activation(out=sg[:, 0:n], in_=pg[:, 0:n], func=AF.Silu),
                             reads=[b_pg], writes=[b_sg])
                        P.op("dve", lambda e, sg=sg, pu=pu, n=n, fc=fc, c0=c0: e.tensor_tensor(
                            out=act[:, fc, c0:c0 + n], in0=sg[:, 0:n], in1=pu[:, 0:n], op=ALU.mult),
                            reads=[b_sg, b_pu], writes=[b_act[fc]])
            wds = []
            dn = w_down[ex].rearrange("(fc p) d -> p fc d", p=128)
            for (f0, nf) in fgroups:
                wds.append(self.wload(dn[:, f0:f0 + nf, :], nf, D))
            for s in subs:
                c0, nt = SUBS[s]
                x = self.xres[s]
                for dh in range(2):
                    ps, b_ps = self.po.next()
                    for gi, (f0, nf) in enumerate(fgroups):
                        wd, b_wd = wds[gi]
                        for j in range(nf):
                            fc = f0 + j
                            P.op("pe", lambda e, ps=ps, fc=fc, j=j, wd=wd, c0=c0, nt=nt, dh=dh: e.matmul(
                                ps[0:nt, :], lhsT=act[:, fc, c0:c0 + nt], rhs=wd[:, j, dh * 512:(dh + 1) * 512],
                                start=(fc == 0), stop=(fc == NFC - 1)), reads=[b_act[fc], b_wd], writes=[b_ps])
                    xs = x[0:nt, dh * 512:(dh + 1) * 512]
                    if gates is None:
                        P.op("dve", lambda e, xs=xs, ps=ps, nt=nt: e.tensor_tensor(
                            out=xs, in0=xs, in1=ps[0:nt, :], op=ALU.add),
                            reads=[b_ps, self.b_xres[s]], writes=[self.b_xres[s]])
                    else:
                        gsc = gates[0:nt, s, 16 + ex:17 + ex]
                        P.op("dve", lambda e, xs=xs, ps=ps, nt=nt, gsc=gsc: e.scalar_tensor_tensor(
                            out=xs, in0=ps[0:nt, :], scalar=gsc, in1=xs, op0=ALU.mult, op1=ALU.add),
                            reads=[b_ps, self.b_xres[s], self.b_gates[s]], writes=[self.b_xres[s]])

    def setup_common(self, ident_ap, ones_ap, pos_ap, invf_ap, mem_ap, w_mem_kv, layers):
        P = self.P
        sb = self.sb
        self.b_big = [Buf() for _ in range(NFC)]
        self.sgt = Rot([sb("sgt%d" % i, [128, 512], F32) for i in range(2)])
        self.pT = Rot([sb("pT%d" % i, [128, 1024], BF16) for i in range(2)])
        self.rden = sb("rden", [128, 512], F32)
        self.b_rden = Buf()
        self.gates = sb("gates", [128, 9, 40], F32)
        self.b_gates = [Buf() for _ in range(9)]
        P.dma("pool", self.ident[:], ident_ap, writes=[self.b_ident])
        P.dma("pool", self.ones[:], ones_ap, writes=[self.b_ones])
        nsub = self.nsup * 8
        self.cs = sb("cs", [128, 32, 16], F32)
        self.b_cs = Buf()
        Bg = self.b_big
        ang = self.bigv(0, 1, F32)[:, 0:512].rearrange("p (a b) -> p a b", a=32, b=16)
        kf = self.bigv(1, 1, F32)[:, 0:512].rearrange("p (a b) -> p a b", a=32, b=16)
        ki = self.bigv(2, 1, F32)[:, 0:512].bitcast(I32).rearrange("p (a b) -> p a b", a=32, b=16)
        r3 = self.bigv(3, 1, F32)
        posi = r3[:, 0:32].bitcast(I32)
        posf = r3[:, 32:64]
        invf = r3[:, 64:72]
        b_t = Buf()
        P.dma("sp", posi, pos_ap, writes=[b_t])
        P.dma("sp", invf, invf_ap, writes=[b_t])
        P.op("dve", lambda e: e.tensor_copy(out=posf, in_=posi), reads=[b_t], writes=[b_t])
        for half in range(2):
            P.op("dve", lambda e, half=half: e.tensor_tensor(
                out=ang[:, :, half * 8:(half + 1) * 8],
                in0=posf.unsqueeze(2).broadcast_to([128, 32, 8]),
                in1=invf.unsqueeze(1).broadcast_to([128, 32, 8]), op=ALU.mult),
                reads=[b_t], writes=[b_t])
        P.op("dve", lambda e: e.tensor_scalar(out=ang[:, :, 0:8], in0=ang[:, :, 0:8], scalar1=float(np.pi / 2),
                                              scalar2=None, op0=ALU.add), reads=[b_t], writes=[b_t])
        C1 = 6.28125
        C2 = float(2 * np.pi - 6.28125)
        P.op("dve", lambda e: e.tensor_scalar(out=kf[:, :, :], in0=ang[:, :, :], scalar1=float(1.0 / (2 * np.pi)),
                                              scalar2=None, op0=ALU.mult), reads=[b_t], writes=[b_t])
        P.op("dve", lambda e: e.tensor_copy(out=ki[:, :, :], in_=kf[:, :, :]), reads=[b_t], writes=[b_t])
        P.op("dve", lambda e: e.tensor_copy(out=kf[:, :, :], in_=ki[:, :, :]), reads=[b_t], writes=[b_t])
        P.op("dve", lambda e: e.scalar_tensor_tensor(out=ang[:, :, :], in0=kf[:, :, :], scalar=-C1, in1=ang[:, :, :],
                                                     op0=ALU.mult, op1=ALU.add), reads=[b_t], writes=[b_t])
        P.op("dve", lambda e: e.scalar_tensor_tensor(out=ang[:, :, :], in0=kf[:, :, :], scalar=-C2, in1=ang[:, :, :],
                                                     op0=ALU.mult, op1=ALU.add), reads=[b_t], writes=[b_t])
        P.op("dve", lambda e: e.tensor_scalar(out=kf[:, :, :], in0=ang[:, :, :], scalar1=float(np.pi),
                                              scalar2=float(2 * np.pi), op0=ALU.is_gt, op1=ALU.mult),
             reads=[b_t], writes=[b_t])
        P.op("dve", lambda e: e.tensor_tensor(out=ang[:, :, :], in0=ang[:, :, :], in1=kf[:, :, :], op=ALU.subtract),
             reads=[b_t], writes=[b_t])
        P.op("dve", lambda e: e.tensor_scalar(out=ang[:, :, :], in0=ang[:, :, :], scalar1=-3.141592, scalar2=3.141592,
                                              op0=ALU.max, op1=ALU.min), reads=[b_t], writes=[b_t])
        P.op("act", lambda e: e.activation(out=self.cs[:, :, :], in_=ang[:, :, :], func=AF.Sin),
             reads=[b_t], writes=[self.b_cs] + Bg[0:4])
        self.memKT = sb("memKT", [128, 2, 2, MEML], BF16)
        self.memV = sb("memV", [128, 2, 2, MEMW], BF16)
        self.b_memkv = Buf()
        self.setup_mem(mem_ap, w_mem_kv, layers)

    def setup_mem(self, mem_ap, w_mem_kv, layers):
        P = self.P
        Bg = self.b_big
        memT = self.bigv(4, 2, BF16)[:, 0:8 * MEML].rearrange("p (a b) -> p a b", a=8, b=MEML)
        b_memT = Buf()
        for mc in range(2):
            xb, b_xb = self.xbf.next()
            P.dma("pool", xb[:], mem_ap[mc * 128:(mc + 1) * 128, :], writes=[b_xb])
            for half in range(2):
                tp, b_tp = self.tp.next()
                for j in range(4):
                    kc = half * 4 + j
                    P.op("pe", lambda e, kc=kc, j=j, tp=tp, xb=xb: e.transpose(
                        out=tp[:, j * 128:(j + 1) * 128], in_=xb[:, kc * 128:(kc + 1) * 128], identity=self.ident[:]),
                        reads=[b_xb, self.b_ident], writes=[b_tp])
                P.op("dve", lambda e, tp=tp, half=half, mc=mc: e.tensor_copy(
                    out=memT[:, half * 4:(half + 1) * 4, mc * 128:(mc + 1) * 128],
                    in_=tp[:, 0:512].rearrange("p (a b) -> p a b", a=4, b=128)),
                    reads=[b_tp], writes=[b_memT] + Bg[4:6])
        for li, l in enumerate(layers):
            w, b_w = self.wload(w_mem_kv[l].rearrange("(kc p) e -> p kc e", p=128), 8, 512)
            for j in range(2):
                ps, b_ps = self.mm.next()
                for kc in range(8):
                    P.op("pe", lambda e, kc=kc, ps=ps, j=j, w=w: e.matmul(
                        ps[:, 0:MEML], lhsT=w[:, kc, j * 128:(j + 1) * 128], rhs=memT[:, kc, :],
                        start=(kc == 0), stop=(kc == 7)), reads=[b_w, b_memT], writes=[b_ps])
                P.op("dve", lambda e, ps=ps, li=li, j=j: e.tensor_copy(out=self.memKT[:, li, j, :], in_=ps[:, 0:MEML]),
                     reads=[b_ps], writes=[self.b_memkv])
            for mc in range(2):
                ps, b_ps = self.mm.next()
                for kc in range(8):
                    P.op("pe", lambda e, kc=kc, ps=ps, mc=mc, w=w: e.matmul(
                        ps[:, 0:MEMW], lhsT=memT[:, kc, mc * 128:(mc + 1) * 128], rhs=w[:, kc, 256:512],
                        start=(kc == 0), stop=(kc == 7)), reads=[b_w, b_memT], writes=[b_ps])
                P.op("dve", lambda e, ps=ps, li=li, mc=mc: e.tensor_copy(out=self.memV[:, li, mc, :], in_=ps[:, 0:MEMW]),
                     reads=[b_ps], writes=[self.b_memkv] + Bg[4:6])

    def bigv(self, r0, nreg, dt):
        v = self.big[:, r0 * NT:(r0 + nreg) * NT]
        if dt == F32:
            v = v.bitcast(F32)
        return v

    def mem_attn(self, li, qm, b_qm, catT, cgs, cb0=0):
        P = self.P
        for h in range(4):
            hp, jj = h % 2, h // 2
            pr = slice(64 * hp, 64 * hp + 64)
            for (c0, n) in cgs:
                pt, b_pt = self.pT.next()
                for mc in range(2):
                    ps, b_ps = self.mm.next()
                    P.op("pe", lambda e, ps=ps, mc=mc, n=n, c0=c0, pr=pr, jj=jj: e.matmul(
                        ps[:, 0:n], lhsT=self.memKT[pr, li, jj, mc * 128:(mc + 1) * 128], rhs=qm[pr, jj, c0:c0 + n],
                        start=True, stop=True), reads=[self.b_memkv] + b_qm, writes=[b_ps])
                    P.op("act", lambda e, ps=ps, mc=mc, n=n, pt=pt: e.activation(
                        out=pt[:, mc * 512:mc * 512 + n], in_=ps[:, 0:n], func=AF.Exp, scale=0.125),
                        reads=[b_ps], writes=[b_pt])
                pn, b_pn = self.mm.next()
                pd, b_pd = self.mm.next()
                for mc in range(2):
                    P.op("pe", lambda e, pn=pn, mc=mc, n=n, pt=pt, pr=pr, h=h: e.matmul(
                        pn[pr, 0:n], lhsT=self.memV[:, li, mc, h * 64:(h + 1) * 64], rhs=pt[:, mc * 512:mc * 512 + n],
                        start=(mc == 0), stop=(mc == 1)), reads=[self.b_memkv, b_pt], writes=[b_pn])
                for mc in range(2):
                    P.op("pe", lambda e, pd=pd, mc=mc, n=n, pt=pt, pr=pr: e.matmul(
                        pd[pr, 0:n], lhsT=self.ones[:, :], rhs=pt[:, mc * 512:mc * 512 + n],
                        start=(mc == 0), stop=(mc == 1)), reads=[self.b_ones, b_pt], writes=[b_pd])
                P.op("dve", lambda e, pd=pd, n=n, pr=pr: e.reciprocal(out=self.rden[pr, 0:n], in_=pd[pr, 0:n]),
                     reads=[b_pd], writes=[self.b_rden])
                P.op("dve", lambda e, pn=pn, n=n, pr=pr, c0=c0, jj=jj: e.tensor_tensor(
                    out=catT[pr, 6 + jj, c0:c0 + n], in0=self.rden[pr, 0:n], in1=pn[pr, 0:n], op=ALU.mult),
                    reads=[b_pn, self.b_rden], writes=[self.b_big[cb0 + 6 + jj]])

    def out_proj_ln(self, w_o_l, catT, subs, which, cb0=0):
        P = self.P
        wo = [self.wload(w_o_l.rearrange("(kc p) d -> p kc d", p=128)[:, :, dh * 512:(dh + 1) * 512], 8, 512)
              for dh in range(2)]
        for s in subs:
            c0, nt = SUBS[s]
            x = self.xres[s]
            for dh in range(2):
                ps, b_ps = self.po.next()
                w, b_w = wo[dh]
                for ec in range(8):
                    P.op("pe", lambda e, ps=ps, ec=ec, w=w, c0=c0, nt=nt: e.matmul(
                        ps[0:nt, :], lhsT=catT[:, ec, c0:c0 + nt], rhs=w[:, ec, :],
                        start=(ec == 0), stop=(ec == 7)), reads=[self.b_big[cb0 + ec], b_w], writes=[b_ps])
                xs = x[0:nt, dh * 512:(dh + 1) * 512]
                P.op("dve", lambda e, xs=xs, ps=ps, nt=nt: e.scalar_tensor_tensor(
                    out=xs, in0=xs, scalar=ALPHA, in1=ps[0:nt, :], op0=ALU.mult, op1=ALU.add),
                    reads=[b_ps, self.b_xres[s]], writes=[self.b_xres[s]])
            self.layer_norm(s, which)
            self.make_xT(s)

    def mixer_a(self, li, w_in_l, w_o_l, convw, b_convw, hfl, b_hfl, subs_out):
        P = self.P
        catT = self.bigv(0, 8, BF16).rearrange("p (a b) -> p a b", a=8, b=NT)
        qm = self.bigv(8, 2, BF16).rearrange("p (a b) -> p a b", a=2, b=NT)
        ubt = self.bigv(10, 2, F32)
        uct = self.bigv(12, 2, F32)
        zct = self.bigv(14, 2, F32)
        zt = self.bigv(16, 2, F32)
        B = self.b_big
        b_ub, b_uc, b_zc, b_z = [B[10], B[11]], [B[12], B[13]], [B[14], B[15]], [B[16], B[17]]
        win = w_in_l.rearrange("(kc p) e -> p kc e", p=128)
        for c in range(6):
            t, b_w = self.ring.next()
            w = t[:, 0:8 * 384].rearrange("p (a b) -> p a b", a=8, b=384)
            for r in range(3):
                P.dma("pool", w[:, :, r * 128:(r + 1) * 128], win[:, :, r * MIXW + c * 128:r * MIXW + (c + 1) * 128],
                      writes=[b_w])
            for r in range(3):
                for (c0, n) in CG_ALL:
                    ps, b_ps = self.mm.next()
                    xb = self.xT_bufs(c0, n)
                    for kc in range(8):
                        P.op("pe", lambda e, kc=kc, ps=ps, r=r, c0=c0, n=n, w=w: e.matmul(
                            ps[:, 0:n], lhsT=w[:, kc, r * 128:(r + 1) * 128], rhs=self.xT[:, kc, c0:c0 + n],
                            start=(kc == 0), stop=(kc == 7)), reads=xb + [b_w], writes=[b_ps])
                    if r == 0:
                        P.op("act", lambda e, ps=ps, c0=c0, n=n: e.activation(out=ubt[:, c0:c0 + n], in_=ps[:, 0:n], func=AF.Copy),
                             reads=[b_ps], writes=b_ub)
                    elif r == 1:
                        P.op("act", lambda e, ps=ps, c0=c0, n=n: e.activation(out=uct[:, c0:c0 + n], in_=ps[:, 0:n], func=AF.Copy),
                             reads=[b_ps], writes=b_uc)
                    else:
                        P.op("dve", lambda e, ps=ps, c0=c0, n=n: e.tensor_tensor(
                            out=zct[:, c0:c0 + n], in0=uct[:, c0:c0 + n], in1=ps[:, 0:n], op=ALU.mult),
                            reads=[b_ps] + b_uc, writes=b_zc)
            P.op("dve", lambda e: e.tensor_tensor(out=zct[:, MAINC:NT], in0=zct[:, MAINC:NT], in1=hfl[:, :], op=ALU.mult),
                 reads=b_zc + [b_hfl], writes=b_zc)
            w0 = convw[:, li, c * 3 + 0:c * 3 + 1]
            w1 = convw[:, li, c * 3 + 1:c * 3 + 2]
            w2 = convw[:, li, c * 3 + 2:c * 3 + 3]
            zm = zt[:, 0:MAINC].rearrange("p (a b) -> p a b", a=4, b=BLK)
            zcm = zct[:, 0:MAINC].rearrange("p (a b) -> p a b", a=4, b=BLK)
            zh = zt[:, MAINC:NT].rearrange("p (a b) -> p a b", a=4, b=4)
            zch = zct[:, MAINC:NT].rearrange("p (a b) -> p a b", a=4, b=4)
            rw = b_zc + [b_convw]
            P.op("pool", lambda e, w2=w2: e.tensor_scalar(out=zt[:, :], in0=zct[:, :], scalar1=w2, scalar2=None, op0=ALU.mult),
                 reads=rw, writes=b_z)

            def stt(o, i0, sc):
                P.op("dve", lambda e: e.scalar_tensor_tensor(out=o, in0=i0, scalar=sc, in1=o, op0=ALU.mult, op1=ALU.add),
                     reads=rw + b_z, writes=b_z)
            stt(zm[:, :, 1:BLK], zcm[:, :, 0:BLK - 1], w1)
            stt(zm[:, :, 2:BLK], zcm[:, :, 0:BLK - 2], w0)
            stt(zm[:, :, 0:1], zch[:, :, 3:4], w1)
            stt(zm[:, :, 0:1], zch[:, :, 2:3], w0)
            stt(zm[:, :, 1:2], zch[:, :, 3:4], w0)
            stt(zh[:, :, 2:4], zch[:, :, 1:3], w1)
            stt(zh[:, :, 2:4], zch[:, :, 0:2], w0)
            P.op("dve", lambda e, c=c: e.tensor_tensor(out=catT[:, c, :], in0=ubt[:, :], in1=zt[:, :], op=ALU.mult),
                 reads=b_ub + b_z, writes=[B[c]])
        wq, b_wq = self.wload(win[:, :, 3 * MIXW:3 * MIXW + MEMW], 8, MEMW)
        for jj in range(2):
            for (c0, n) in CG_ALL:
                ps, b_ps = self.mm.next()
                xb = self.xT_bufs(c0, n)
                for kc in range(8):
                    P.op("pe", lambda e, kc=kc, ps=ps, jj=jj, c0=c0, n=n: e.matmul(
                        ps[:, 0:n], lhsT=wq[:, kc, jj * 128:(jj + 1) * 128], rhs=self.xT[:, kc, c0:c0 + n],
                        start=(kc == 0), stop=(kc == 7)), reads=xb + [b_wq], writes=[b_ps])
                P.op("act", lambda e, ps=ps, jj=jj, c0=c0, n=n: e.activation(out=qm[:, jj, c0:c0 + n], in_=ps[:, 0:n], func=AF.Copy),
                     reads=[b_ps], writes=[B[8 + jj]])
        self.mem_attn(li, qm, [B[8], B[9]], catT, CG_ALL)
        self.out_proj_ln(w_o_l, catT, subs_out, 0)

    def kv_proj(self, w_kv, sup, kT_loc, v_loc):
        P = self.P
        B = self.b_big
        wv = w_kv.rearrange("(kc p) e -> p kc e", p=128)
        w = [self.wload(wv[:, :, i * 512:(i + 1) * 512], 8, 512) for i in range(3)]
        ktok = self.bigv(0, 2, F32)[:, 0:MIXW]
        kbf = self.bigv(2, 1, BF16)[:, 0:MIXW]
        vbf = self.bigv(3, 1, BF16)[:, 0:MIXW]
        kTt = self.bigv(4, 6, BF16)[:, 0:6 * MAINC].rearrange("p (a b) -> p a b", a=6, b=MAINC)
        b_kT = B[4:10]
        b_k, b_kb, b_v = [B[0], B[1]], [B[2]], [B[3]]
        k3 = ktok.rearrange("p (h d) -> p h d", h=12, d=64)
        rt = self.rtmp
        b_rt = self.b_rtmp
        for s in range(8):
            c0, nt = SUBS[s]
            gs = sup * 8 + s
            pss = []
            for i in range(3):
                ps, b_ps = (self.mm.next() if i < 2 else self.po.next())
                for kc in range(8):
                    P.op("pe", lambda e, ps=ps, kc=kc, i=i, c0=c0: e.matmul(
                        ps[:, :], lhsT=self.xT[:, kc, c0:c0 + 128], rhs=w[i][0][:, kc, :],
                        start=(kc == 0), stop=(kc == 7)), reads=[self.b_xT[s], w[i][1]], writes=[b_ps])
                pss.append((ps, b_ps))
            P.op("act", lambda e, ps=pss[0][0]: e.activation(out=ktok[:, 0:512], in_=ps[:, :], func=AF.Copy),
                 reads=[pss[0][1]], writes=b_k)
            P.op("act", lambda e, ps=pss[1][0]: e.activation(out=ktok[:, 512:768], in_=ps[:, 0:256], func=AF.Copy),
                 reads=[pss[1][1]], writes=b_k)
            P.op("dve", lambda e, ps=pss[1][0]: e.tensor_copy(out=vbf[:, 0:256], in_=ps[:, 256:512]),
                 reads=[pss[1][1]], writes=b_v)
            P.op("dve", lambda e, ps=pss[2][0]: e.tensor_copy(out=vbf[:, 256:768], in_=ps[:, :]),
                 reads=[pss[2][1]], writes=b_v)
            self.kv_writes.append(P.dma(
                "sp", v_loc.rearrange("(h p) (t d) -> p h t d", p=128, d=64)[:, :, gs, :],
                vbf.rearrange("p (h d) -> p h d", h=12, d=64), reads=b_v))
            self.rope(k3, b_k, gs, 12)
            P.op("act", lambda e: e.activation(out=kbf, in_=ktok, func=AF.Copy), reads=b_k, writes=b_kb)
            for half in range(2):
                tp, b_tp = self.tp.next()
                for j in range(3):
                    hp = half * 3 + j
                    P.op("pe", lambda e, tp=tp, j=j, hp=hp: e.transpose(
                        out=tp[:, j * 128:(j + 1) * 128], in_=kbf[:, hp * 128:(hp + 1) * 128], identity=self.ident[:]),
                        reads=b_kb + [self.b_ident], writes=[b_tp])
                P.op("dve", lambda e, tp=tp, half=half, c0=c0: e.tensor_copy(
                    out=kTt[:, half * 3:(half + 1) * 3, c0:c0 + 128],
                    in_=tp[:, 0:384].rearrange("p (a b) -> p a b", a=3, b=128)),
                    reads=[b_tp], writes=b_kT[half * 3:(half + 1) * 3])
        self.kv_writes.append(P.dma(
            "sp", kT_loc.rearrange("(h p) t -> p h t", p=128)[:, :, sup * MAINC:(sup + 1) * MAINC], kTt, reads=b_kT))
        for hp in range(6):
            P.op("dve", lambda e, hp=hp: e.tensor_reduce(
                out=self.kmT[:, hp, sup * 4:(sup + 1) * 4],
                in_=kTt[:, hp, :].rearrange("p (a b) -> p a b", a=4, b=BLK),
                axis=mybir.AxisListType.X, op=ALU.add), reads=b_kT, writes=[self.b_kmT])

    def rope(self, t3, b_t, gs, nh):
        P = self.P
        rt, b_rt = self.rtmp, self.b_rtmp
        cos = self.cs[:, gs, 0:8].unsqueeze(1).broadcast_to([128, nh, 8])
        sin = self.cs[:, gs, 8:16].unsqueeze(1).broadcast_to([128, nh, 8])
        t1 = t3[:, :, 0:8]
        t2 = t3[:, :, 8:16]
        rd = b_t + [self.b_cs]
        P.op("dve", lambda e: e.tensor_tensor(out=rt[:, 0, 0:nh, :], in0=t1, in1=cos, op=ALU.mult), reads=rd, writes=[b_rt])
        P.op("dve", lambda e: e.tensor_tensor(out=rt[:, 1, 0:nh, :], in0=t2, in1=sin, op=ALU.mult), reads=rd, writes=[b_rt])
        P.op("dve", lambda e: e.tensor_tensor(out=rt[:, 2, 0:nh, :], in0=t2, in1=cos, op=ALU.mult), reads=rd, writes=[b_rt])
        P.op("dve", lambda e: e.tensor_tensor(out=rt[:, 3, 0:nh, :], in0=t1, in1=sin, op=ALU.mult), reads=rd, writes=[b_rt])
        P.op("dve", lambda e: e.tensor_tensor(out=t1, in0=rt[:, 0, 0:nh, :], in1=rt[:, 1, 0:nh, :], op=ALU.subtract),
             reads=[b_rt], writes=b_t)
        P.op("dve", lambda e: e.tensor_tensor(out=t2, in0=rt[:, 2, 0:nh, :], in1=rt[:, 3, 0:nh, :], op=ALU.add),
             reads=[b_rt], writes=b_t)


WSHAPES = {
    "w_in_a": ([2, D, 2560], F32), "w_q_b": ([2, D, D], F32), "w_kv_shared": ([D, 1536], F32),
    "w_mem_kv": ([4, D, 512], F32), "w_o": ([4, D, D], F32),
    "ln1_g": ([4, D], F32), "ln1_b": ([4, D], F32), "ln2_g": ([4, D], F32), "ln2_b": ([4, D], F32),
    "w_gu_dense0": ([D, 2 * DFF], F32), "w_down_dense0": ([DFF, D], F32),
    "w_gu_dense1": ([D, 2 * DFF], F32), "w_down_dense1": ([DFF, D], F32), "w_router": ([2, D, NEXP], F32),
    "w_gu_moe0": ([NEXP, D, 2 * DFF], F32), "w_down_moe0": ([NEXP, DFF, D], F32),
    "w_gu_moe1": ([NEXP, D, 2 * DFF], F32), "w_down_moe1": ([NEXP, DFF, D], F32),
    "ident": ([128, 128], F32), "ones": ([128, 64], F32), "invf": ([128, 8], F32), "conv_r": ([128, 2, 18], F32),
    "pos_t": ([128, 32], I32), "mem": ([MEML, D], F32),
}


class LazyW(dict):
    def __init__(self, nc):
        super().__init__()
        self.nc = nc

    def __missing__(self, name):
        shape, dt = WSHAPES[name]
        ap = self.nc.dram_tensor(name, shape, dt, kind="ExternalInput").ap()
        self[name] = ap
        return ap


def declare_weights(nc):
    return LazyW(nc)


def ffn_layer(k, W, layer, subs, cgs):
    if layer % 2 == 0:
        k.ffn([W["w_gu_dense%d" % (layer // 2)]], [W["w_down_dense%d" % (layer // 2)]], subs, cgs)
    else:
        m = layer // 2
        k.ffn([W["w_gu_moe%d" % m][e] for e in range(NEXP)], [W["w_down_moe%d" % m][e] for e in range(NEXP)],
              subs, cgs, w_router=W["w_router"][m])


def emit_phase_a(k, W, x_main, x_halo, hflag, x2s, kT_loc, v_loc, km_loc, nsup, stage=99):
    P = k.P
    k.kv_writes = []
    x2w = {}
    k.kmT = k.sb("kmT_sb", [128, 6, NLOC], F32)
    k.b_kmT = Buf()
    convw = k.sb("convw", [128, 2, 18], F32)
    b_convw = Buf()
    P.dma("sp", convw[:], W["conv_r"], writes=[b_convw])
    hfl = k.sb("hfl", [128, 16], F32)
    b_hfl = Buf()
    ALLS = list(range(9))
    MAINS = list(range(8))
    for sup in range(nsup):
        P.dma("sp", hfl[:], hflag[0:1, sup * 16:(sup + 1) * 16].broadcast_to([128, 16]), writes=[b_hfl])
        for s in range(9):
            c0, nt = SUBS[s]
            src = x_main[sup * MAINC + c0:sup * MAINC + c0 + 128, :] if s < 8 else x_halo[sup * 16:(sup + 1) * 16, :]
            P.dma("sp", k.xres[s][0:nt, :], src, writes=[k.b_xres[s]])
            k.make_xT(s)
        for layer in (0, 1):
            if stage < 1 + 2 * layer:
                break
            k.load_ln(W["ln1_g"][layer:layer + 1, :], W["ln1_b"][layer:layer + 1, :], 0)
            k.load_ln(W["ln2_g"][layer:layer + 1, :], W["ln2_b"][layer:layer + 1, :], 1)
            subs = ALLS if layer == 0 else MAINS
            cgs = CG_ALL if layer == 0 else CG_MAIN
            k.mixer_a(layer, W["w_in_a"][layer], W["w_o"][layer], convw, b_convw, hfl, b_hfl, subs)
            if stage < 2 + 2 * layer:
                break
            ffn_layer(k, W, layer, subs, cgs)
            for s in subs:
                k.layer_norm(s, 1)
                k.make_xT(s)
        for s in MAINS:
            c0, nt = SUBS[s]
            x2w[(sup, s)] = P.dma("sp", x2s[sup * MAINC + c0:sup * MAINC + c0 + 128, :], k.xres[s][:, :],
                                  reads=[k.b_xres[s]])
        if stage >= 5:
            k.kv_proj(W["w_kv_shared"], sup, kT_loc, v_loc)
    P.op("dve", lambda e: e.tensor_scalar(out=k.kmT[:, :, :], in0=k.kmT[:, :, :], scalar1=1.0 / BLK, scalar2=None,
                                          op0=ALU.mult), reads=[k.b_kmT], writes=[k.b_kmT])
    k.kv_writes.append(P.dma("sp", km_loc.rearrange("(h p) n -> p h n", p=128), k.kmT[:, :, :], reads=[k.b_kmT]))
    return x2w, k.kv_writes


def new_k(nc, st, nsup, W, layers):
    k = K(nc, st, nsup)
    k.setup_common(W["ident"], W["ones"], W["pos_t"], W["invf"], W["mem"], W["w_mem_kv"], layers)
    k.rtmp = k.sb("rtmp", [128, 4, 12, 8], F32)
    k.b_rtmp = Buf()
    return k


def build_phase_a(nsup=NSUP, stage=99):
    nc = bass.Bass("TRN2", target_bir_lowering=False)
    W = declare_weights(nc)
    x_main = nc.dram_tensor("x_main", [NLOC * BLK, D], F32, kind="ExternalInput").ap()
    x_halo = nc.dram_tensor("x_halo", [NLOC * 4, D], F32, kind="ExternalInput").ap()
    hflag = nc.dram_tensor("hflag", [1, NLOC * 4], F32, kind="ExternalInput").ap()
    x2 = nc.dram_tensor("x2", [NLOC * BLK, D], F32, kind="ExternalOutput").ap()
    kT = nc.dram_tensor("kT", [6 * 128, NLOC * BLK], BF16, kind="ExternalOutput").ap()
    v = nc.dram_tensor("v", [12 * 128, 32 * 64], BF16, kind="ExternalOutput").ap()
    kmT = nc.dram_tensor("kmT", [6 * 128, NLOC], F32, kind="ExternalOutput").ap()
    with ExitStack() as st:
        k = new_k(nc, st, nsup, W, (0, 1))
        emit_phase_a(k, W, x_main, x_halo, hflag, x2, kT, v, kmT, nsup, stage)
        outs = [o for o in k.P.q["sp"] if o.is_dma][-(NDMA_SEM):]
        k.P.emit(final_waits=outs)
    nc.used_inputs = list(W.keys())
    return nc


def host_consts():
    invf = np.power(np.float32(500000.0), -np.arange(0, 16, 2, dtype=np.float32) / np.float32(16)).astype(np.float32)
    return {
        "ident": np.eye(128, dtype=np.float32),
        "ones": np.ones((128, 64), np.float32),
        "invf": np.ascontiguousarray(np.broadcast_to(invf[None, :], (128, 8))),
    }


def core_layout(c):
    b, h = divmod(c, 2)
    gl = [glob_block(i, h) for i in range(NLOC)]
    return b, h, gl


def phase_a_inputs(inp, c):
    b, h, gl = core_layout(c)
    x = inp["x"][b]
    x_main = np.concatenate([x[g * BLK:(g + 1) * BLK] for g in gl], axis=0)
    halo = []
    flags = []
    for g in gl:
        if g == 0:
            halo.append(np.zeros((4, D), np.float32)); flags += [0.0] * 4
        else:
            halo.append(x[g * BLK - 4:g * BLK]); flags += [1.0] * 4
    pos = inp["positions"][b]
    pos_loc = np.concatenate([pos[g * BLK:(g + 1) * BLK] for g in gl]).astype(np.int32)
    d = {
        "x_main": np.ascontiguousarray(x_main), "x_halo": np.ascontiguousarray(np.concatenate(halo, axis=0)),
        "hflag": np.asarray(flags, np.float32)[None, :],
        "pos_t": np.ascontiguousarray(pos_loc.reshape(32, 128).T),
        "mem": np.ascontiguousarray(inp["mem"][b]),
        "conv_r": np.ascontiguousarray(inp["conv_a"].reshape(2, 3, 6, 128).transpose(3, 0, 2, 1).reshape(128, 2, 18)),
    }
    return d


WNAMES = ["w_in_a", "w_q_b", "w_kv_shared", "w_mem_kv", "w_o", "ln1_g", "ln1_b", "ln2_g", "ln2_b", "w_router"]


def weight_map(inp, used):
    d = {}
    for n in WNAMES:
        if n in used:
            d[n] = inp[n]
    for m in range(2):
        for base in ("w_gu_dense", "w_down_dense", "w_gu_moe", "w_down_moe"):
            n = "%s%d" % (base, m)
            if n in used:
                d[n] = inp[base][m]
    return d


def mixer_b(k, li, sup, w_q_l, w_o_l, kT_all, v_all, kT_loc, v_loc, blkind, kvdeps):
    P = k.P
    B = k.b_big
    Qaug = k.bigv(0, 12, BF16).rearrange("p (a b) -> p a b", a=12, b=NT)
    qm = k.bigv(12, 2, BF16).rearrange("p (a b) -> p a b", a=2, b=NT)
    catT = k.bigv(14, 8, BF16).rearrange("p (a b) -> p a b", a=8, b=NT)
    b_Q = B[0:12]
    wqv = w_q_l.rearrange("(kc p) e -> p kc e", p=128)
    wq = [k.wload(wqv[:, :, i * 512:(i + 1) * 512], 8, 512) for i in range(2)]
    qtok = k.xres[8]
    b_qtok = [k.b_xres[8]]
    q3 = qtok[:, 0:MIXW].rearrange("p (h d) -> p h d", h=12, d=64)
    for s in range(8):
        c0, nt = SUBS[s]
        gs = sup * 8 + s
        lb = sup * 4 + s // 2
        for half in range(2):
            ps, b_ps = k.mm.next()
            for kc in range(8):
                P.op("pe", lambda e, ps=ps, kc=kc, half=half, c0=c0: e.matmul(
                    ps[:, :], lhsT=k.xT[:, kc, c0:c0 + 128], rhs=wq[half][0][:, kc, :],
                    start=(kc == 0), stop=(kc == 7)), reads=[k.b_xT[s], wq[half][1]], writes=[b_ps])
            P.op("act", lambda e, ps=ps, half=half: e.activation(out=qtok[:, half * 512:(half + 1) * 512], in_=ps[:, :], func=AF.Copy),
                 reads=[b_ps], writes=b_qtok)
        k.rope(q3, b_qtok, gs, 12)
        qbf, b_qbf = k.xbf.next()
        P.op("act", lambda e, qbf=qbf: e.activation(out=qbf[:, :], in_=qtok[:, :], func=AF.Copy), reads=b_qtok, writes=[b_qbf])
        tpA, b_tpA = k.tp.next()
        for h in range(8):
            P.op("pe", lambda e, h=h, tpA=tpA, qbf=qbf: e.transpose(
                out=tpA[0:64, h * 128:(h + 1) * 128], in_=qbf[:, h * 64:(h + 1) * 64], identity=k.ident[:]),
                reads=[b_qbf, k.b_ident], writes=[b_tpA])
        P.op("dve", lambda e, tpA=tpA, c0=c0: e.tensor_copy(
            out=Qaug[0:64, 0:8, c0:c0 + 128], in_=tpA[0:64, 0:1024].rearrange("p (a b) -> p a b", a=8, b=128)),
            reads=[b_tpA], writes=b_Q[0:8])
        tpB, b_tpB = k.tp.next()
        for h in range(8, 12):
            P.op("pe", lambda e, h=h, tpB=tpB, qbf=qbf: e.transpose(
                out=tpB[0:64, (h - 8) * 128:(h - 7) * 128], in_=qbf[:, h * 64:(h + 1) * 64], identity=k.ident[:]),
                reads=[b_qbf, k.b_ident], writes=[b_tpB])
        for jj in range(2):
            P.op("pe", lambda e, jj=jj, tpB=tpB, qbf=qbf: e.transpose(
                out=tpB[:, 512 + jj * 128:640 + jj * 128], in_=qbf[:, MIXW + jj * 128:MIXW + (jj + 1) * 128], identity=k.ident[:]),
                reads=[b_qbf, k.b_ident], writes=[b_tpB])
        P.op("act", lambda e, tpB=tpB, c0=c0: e.activation(
            out=Qaug[0:64, 8:12, c0:c0 + 128], in_=tpB[0:64, 0:512].rearrange("p (a b) -> p a b", a=4, b=128), func=AF.Copy),
            reads=[b_tpB], writes=b_Q[8:12])
        P.op("dve", lambda e, tpB=tpB, c0=c0: e.tensor_copy(
            out=qm[:, :, c0:c0 + 128], in_=tpB[:, 512:768].rearrange("p (a b) -> p a b", a=2, b=128)),
            reads=[b_tpB], writes=B[12:14])
        pg, b_pg = k.po.next()
        for h in range(12):
            P.op("pe", lambda e, h=h, pg=pg, c0=c0: e.matmul(
                pg[:, h * 32:(h + 1) * 32], lhsT=Qaug[0:64, h, c0:c0 + 128], rhs=k.kmh[0:64, h, :],
                start=True, stop=True), reads=[b_Q[h], k.b_kmh], writes=[b_pg])
        gsb, mx, sel, biasb = k.gsb, k.mx, k.sel, k.biasb
        b_g = k.b_gate
        vb = k.valid_bc[:, lb, :].unsqueeze(1).broadcast_to([128, 12, 32])
        P.op("dve", lambda e, pg=pg, vb=vb: e.scalar_tensor_tensor(
            out=gsb[:, :, :], in0=pg[:, 0:384].rearrange("p (a b) -> p a b", a=12, b=32), scalar=64.0, in1=vb,
            op0=ALU.add, op1=ALU.mult), reads=[b_pg, k.b_vn], writes=[b_g])
        P.op("dve", lambda e: e.tensor_scalar(out=gsb[:, :, :], in0=gsb[:, :, :], scalar1=-64.0, scalar2=None, op0=ALU.add),
             reads=[b_g], writes=[b_g])
        for h in range(12):
            P.op("dve", lambda e, h=h: e.max(out=mx[:, h, :], in_=gsb[:, h, :]), reads=[b_g], writes=[b_g])
        for h in range(12):
            P.op("dve", lambda e, h=h: e.tensor_scalar(out=sel[:, h, :], in0=gsb[:, h, :], scalar1=mx[:, h, 2:3],
                                                       scalar2=None, op0=ALU.is_ge), reads=[b_g], writes=[b_g])
        P.op("dve", lambda e, vb=vb: e.tensor_tensor(out=sel[:, :, :], in0=sel[:, :, :], in1=vb, op=ALU.mult),
             reads=[b_g, k.b_vn], writes=[b_g])
        P.op("dve", lambda e: e.tensor_scalar(out=biasb[:, :, :], in0=sel[:, :, :], scalar1=-NEGBIG, scalar2=NEGBIG,
                                              op0=ALU.mult, op1=ALU.add), reads=[b_g], writes=[b_g])
        for grp, (h0, h1) in enumerate(((0, 8), (8, 12))):
            tpC, b_tpC = k.tp.next()
            for h in range(h0, h1):
                P.op("pe", lambda e, h=h, h0=h0, tpC=tpC: e.transpose(
                    out=tpC[64:96, (h - h0) * 128:(h - h0 + 1) * 128], in_=biasb[:, h, :], identity=k.ident[:]),
                    reads=[b_g, k.b_ident], writes=[b_tpC])
            n = h1 - h0
            P.op("dve", lambda e, tpC=tpC, h0=h0, h1=h1, n=n, c0=c0: e.tensor_copy(
                out=Qaug[64:96, h0:h1, c0:c0 + 128], in_=tpC[64:96, 0:n * 128].rearrange("p (a b) -> p a b", a=n, b=128)),
                reads=[b_tpC], writes=b_Q[h0:h1])
    units = []
    cur = [None]
    for h in range(12):
        hp, ho = h // 2, (h % 2) * 64
        pr = slice(ho, ho + 64)
        hc = {}

        def loader(hc=hc, h=h, hp=hp, ho=ho):
            if hc:
                return
            kslots = []
            for r in range(2):
                t, b = k.ring.next()
                P.dma("sp", t[0:64, 0:4096], kT_all[r * 768 + hp * 128 + ho:r * 768 + hp * 128 + ho + 64, :],
                      writes=[b], deps=kvdeps)
                P.dma("sp", t[64:96, 0:4096], blkind[r], writes=[b])
                kslots.append((t, b))
            t, b_v = k.ring.next()
            vt = t[:, 0:4096].rearrange("p (a b) -> p a b", a=64, b=64)
            for r in range(2):
                P.dma("sp", vt[:, r * 32:(r + 1) * 32, :],
                      v_all[r * 1536 + h * 128:r * 1536 + (h + 1) * 128, :].rearrange("p (t d) -> p t d", d=64),
                      writes=[b_v], deps=kvdeps)
            okt, b_ok = k.okt.next()
            P.dma("sp", okt[0:64, :], kT_loc[hp * 128 + ho:hp * 128 + ho + 64, sup * MAINC:(sup + 1) * MAINC],
                  writes=[b_ok], deps=kvdeps)
            ovt, b_ov = k.ovt.next()
            P.dma("sp", ovt[:, :, :],
                  v_loc[h * 128:(h + 1) * 128, :].rearrange("p (t d) -> p t d", d=64)[:, sup * 8:(sup + 1) * 8, :],
                  writes=[b_ov], deps=kvdeps)
            hc.update(kslots=kslots, vt=vt, b_v=b_v, okt=okt, b_ok=b_ok, ovt=ovt, b_ov=b_ov)
        for bi in range(4):
            i = sup * 4 + bi
            q0 = bi * BLK
            npast = max(glob_block(i, 0), glob_block(i, 1))
            pacc, b_pacc = None, None
            nun = npast + 1
            for u in range(nun):
                own = (u == npast)
                rj, ij = GLOB_INV[u] if not own else (0, 0)
                kc0 = ij * BLK
                vti = rj * 32 + ij * 2

                def stage1(own=own, u=u, rj=rj, kc0=kc0, q0=q0, h=h, bi=bi, hc=hc, loader=loader):
                    loader()
                    okt, b_ok = hc["okt"], hc["b_ok"]
                    kt, b_k = hc["kslots"][rj]
                    S, b_S = k.mm.next()
                    pt, b_pt = k.pt4.next()
                    if not own:
                        for kh in range(2):
                            P.op("pe", lambda e, kh=kh: e.matmul(
                                S[:, kh * BLK:(kh + 1) * BLK], lhsT=kt[0:96, kc0 + kh * 128:kc0 + (kh + 1) * 128],
                                rhs=Qaug[0:96, h, q0:q0 + BLK], start=True, stop=True),
                                reads=[b_k, b_Q[h]], writes=[b_S])
                        P.op("act", lambda e: e.activation(out=pt[:, 0:512], in_=S[:, 0:512], func=AF.Exp, scale=0.125),
                             reads=[b_S], writes=[b_pt])
                    else:
                        P.op("pe", lambda e: e.matmul(
                            S[:, 0:BLK], lhsT=okt[0:64, bi * BLK:bi * BLK + 128],
                            rhs=Qaug[0:64, h, q0:q0 + BLK], start=True, stop=True),
                            reads=[b_ok, b_Q[h]], writes=[b_S])
                        P.op("pe", lambda e: e.matmul(
                            S[:, BLK:BLK + 128], lhsT=okt[0:64, bi * BLK + 128:(bi + 1) * BLK],
                            rhs=Qaug[0:64, h, q0 + 128:q0 + BLK], start=True, stop=True),
                            reads=[b_ok, b_Q[h]], writes=[b_S])
                        P.op("act", lambda e: e.activation(out=pt[:, 0:384], in_=S[:, 0:384], func=AF.Exp, scale=0.125),
                             reads=[b_S], writes=[b_pt])
                        P.op("pool", lambda e: e.tensor_tensor(
                            out=pt[:, 0:512].rearrange("p (a b) -> p a b", a=2, b=BLK)[:, :, 0:128],
                            in0=pt[:, 0:512].rearrange("p (a b) -> p a b", a=2, b=BLK)[:, :, 0:128],
                            in1=k.tri[:, :].unsqueeze(1).broadcast_to([128, 2, 128]), op=ALU.mult),
                            reads=[b_pt, k.b_tri], writes=[b_pt])
                    return pt, b_pt

                def stage2(st1, own=own, u=u, vti=vti, bi=bi, pr=pr, hp=hp, q0=q0, hc=hc):
                    pt, b_pt = st1
                    ovt, b_ov, vt, b_v = hc["ovt"], hc["b_ov"], hc["vt"], hc["b_v"]
                    st = cur[0]
                    if u == 0:
                        st = k.po.next()
                        cur[0] = st
                    pacc, b_pacc = st
                    if not own:
                        for kh in range(2):
                            P.op("pe", lambda e, kh=kh: e.matmul(
                                pacc[pr, 0:BLK], lhsT=vt[:, vti + kh, :], rhs=pt[:, kh * BLK:(kh + 1) * BLK],
                                start=(u == 0 and kh == 0), stop=False), reads=[b_v, b_pt], writes=[b_pacc])
                            P.op("pe", lambda e, kh=kh: e.matmul(
                                pacc[pr, BLK:2 * BLK], lhsT=k.ones[:, :], rhs=pt[:, kh * BLK:(kh + 1) * BLK],
                                start=False, stop=False), reads=[k.b_ones, b_pt], writes=[b_pacc])
                    else:
                        for kh in range(2):
                            qlo = kh * 128
                            n = BLK - qlo
                            P.op("pe", lambda e, kh=kh, qlo=qlo, n=n: e.matmul(
                                pacc[pr, qlo:BLK], lhsT=ovt[:, bi * 2 + kh, :], rhs=pt[:, kh * BLK:kh * BLK + n],
                                start=False, stop=(kh == 1)), reads=[b_ov, b_pt], writes=[b_pacc])
                            P.op("pe", lambda e, kh=kh, qlo=qlo, n=n: e.matmul(
                                pacc[pr, BLK + qlo:2 * BLK], lhsT=k.ones[:, :], rhs=pt[:, kh * BLK:kh * BLK + n],
                                start=False, stop=(kh == 1)), reads=[k.b_ones, b_pt], writes=[b_pacc])
                        P.op("dve", lambda e: e.reciprocal(out=k.rden[pr, 0:BLK], in_=pacc[pr, BLK:2 * BLK]),
                             reads=[b_pacc], writes=[k.b_rden])
                        P.op("dve", lambda e: e.tensor_tensor(
                            out=catT[pr, hp, q0:q0 + BLK], in0=k.rden[pr, 0:BLK], in1=pacc[pr, 0:BLK], op=ALU.mult),
                            reads=[b_pacc, k.b_rden], writes=[B[14 + hp]])

                units.append((stage1, stage2))
    LOOK = 2
    pend = []
    for idx in range(len(units) + LOOK):
        if idx < len(units):
            pend.append(units[idx][0]())
        if idx >= LOOK:
            units[idx - LOOK][1](pend[idx - LOOK])
    k.mem_attn(li, qm, B[12:14], catT, CG_MAIN, cb0=14)
    k.out_proj_ln(w_o_l, catT, list(range(8)), 0, cb0=14)


def emit_phase_b(k, W, x2s, kT_all, v_all, km_all, kT_loc, v_loc, blkind, tri_in, bvalid, y, nsup, x2w, kvdeps, stage=99):
    P = k.P
    k.tri = k.sb("tri", [128, 128], BF16)
    k.b_tri = Buf()
    P.dma("pool", k.tri[:], tri_in, writes=[k.b_tri])
    k.kmh = k.sb("kmh", [64, 12, NBLK], BF16)
    k.b_kmh = Buf()
    for h in range(12):
        for r in range(2):
            row = r * 768 + (h // 2) * 128 + (h % 2) * 64
            P.dma("pool", k.kmh[0:64, h, r * 16:(r + 1) * 16], km_all[row:row + 64, :], writes=[k.b_kmh], deps=kvdeps)
    k.valid_bc = k.sb("valid_bc", [128, NLOC, NBLK], F32)
    k.b_vn = Buf()
    P.dma("sp", k.valid_bc[:, :, :].rearrange("p a b -> p (a b)"), bvalid.broadcast_to([128, NLOC * NBLK]), writes=[k.b_vn])
    k.gsb = k.sb("gsb", [128, 12, 32], F32)
    k.sel = k.sb("sel", [128, 12, 32], F32)
    k.mx = k.sb("mx", [128, 12, 8], F32)
    k.biasb = k.sb("biasb", [128, 12, 32], BF16)
    k.b_gate = Buf()
    k.okt = Rot([k.sb("okt%d" % i, [64, MAINC], BF16) for i in range(1)])
    k.ovt = Rot([k.sb("ovt%d" % i, [128, 8, 64], BF16) for i in range(2)])
    k.pt4 = Rot([k.sb("pt4_%d" % i, [128, 512], BF16) for i in range(3)])
    MAINS = list(range(8))
    outs = []
    for sup in range(nsup):
        for s in MAINS:
            c0, nt = SUBS[s]
            P.dma("sp", k.xres[s][:, :], x2s[sup * MAINC + c0:sup * MAINC + c0 + 128, :], writes=[k.b_xres[s]],
                  deps=[x2w.get((sup, s))])
            k.make_xT(s)
        for layer in (2, 3):
            if stage < 1 + 2 * (layer - 2):
                break
            k.load_ln(W["ln1_g"][layer:layer + 1, :], W["ln1_b"][layer:layer + 1, :], 0)
            k.load_ln(W["ln2_g"][layer:layer + 1, :], W["ln2_b"][layer:layer + 1, :], 1)
            mixer_b(k, layer - 2, sup, W["w_q_b"][layer - 2], W["w_o"][layer], kT_all, v_all, kT_loc, v_loc, blkind, kvdeps)
            if stage < 2 + 2 * (layer - 2):
                break
            ffn_layer(k, W, layer, MAINS, CG_MAIN)
            for s in MAINS:
                k.layer_norm(s, 1)
                if layer == 2:
                    k.make_xT(s)
        for s in MAINS:
            c0, nt = SUBS[s]
            outs.append(P.dma("sp", y[sup * MAINC + c0:sup * MAINC + c0 + 128, :], k.xres[s][:, :], reads=[k.b_xres[s]]))
    return outs


def build_phase_b(nsup=NSUP, stage=99):
    nc = bass.Bass("TRN2", target_bir_lowering=False)
    W = declare_weights(nc)
    di = lambda n, s, d=F32: nc.dram_tensor(n, s, d, kind="ExternalInput").ap()
    x2 = di("x2", [NLOC * BLK, D])
    kT_all = di("kT_all", [2 * 768, NLOC * BLK], BF16)
    v_all = di("v_all", [2 * 1536, 2048], BF16)
    km_all = di("km_all", [2 * 768, NLOC])
    kT_loc = di("kT_loc", [768, NLOC * BLK], BF16)
    v_loc = di("v_loc", [1536, 2048], BF16)
    blkind = di("blkind", [2, 32, 4096], BF16)
    tri_in = di("tri", [128, 128])
    bvalid = di("bvalid", [1, NLOC * NBLK])
    y = nc.dram_tensor("y", [NLOC * BLK, D], F32, kind="ExternalOutput").ap()
    with ExitStack() as st:
        k = new_k(nc, st, nsup, W, (2, 3))
        outs = emit_phase_b(k, W, x2, kT_all, v_all, km_all, kT_loc, v_loc, blkind, tri_in, bvalid, y, nsup, {}, [], stage)
        k.P.emit(final_waits=outs[-NDMA_SEM:])
    nc.used_inputs = list(W.keys())
    return nc


RGROUPS = [[0, 1], [2, 3], [4, 5], [6, 7]]


def build_fused(nsup=NSUP):
    nc = bass.Bass("TRN2", target_bir_lowering=False)
    W = declare_weights(nc)
    di = lambda n, s, d=F32: nc.dram_tensor(n, s, d, kind="ExternalInput").ap()
    x_main = di("x_main", [NLOC * BLK, D])
    x_halo = di("x_halo", [NLOC * 4, D])
    hflag = di("hflag", [1, NLOC * 4])
    blkind = di("blkind", [2, 32, 4096], BF16)
    tri_in = di("tri", [128, 128])
    bvalid = di("bvalid", [1, NLOC * NBLK])
    y = nc.dram_tensor("y", [NLOC * BLK, D], F32, kind="ExternalOutput").ap()
    x2s = nc.dram_tensor("x2s", [NLOC * BLK, D], F32).ap()
    kT_loc = nc.dram_tensor("kT_loc", [768, NLOC * BLK], BF16).ap()
    v_loc = nc.dram_tensor("v_loc", [1536, 2048], BF16).ap()
    km_loc = nc.dram_tensor("km_loc", [768, NLOC], F32).ap()
    kT_all = nc.dram_tensor("kT_all", [2 * 768, NLOC * BLK], BF16).ap()
    v_all = nc.dram_tensor("v_all", [2 * 1536, 2048], BF16).ap()
    km_all = nc.dram_tensor("km_all", [2 * 768, NLOC], F32).ap()
    with ExitStack() as st:
        k = new_k(nc, st, nsup, W, (0, 1))
        P = k.P
        x2w, kvw = emit_phase_a(k, W, x_main, x_halo, hflag, x2s, kT_loc, v_loc, km_loc, nsup)
        gath = []
        for src, dst in ((kT_loc, kT_all), (v_loc, v_all), (km_loc, km_all)):
            gath.append(P.op("pool", lambda e, src=src, dst=dst: e.collective_compute(
                "AllGather", ALU.bypass, replica_groups=RGROUPS, ins=[src], outs=[dst]), deps=kvw, dma=True))
        k.setup_mem(W["mem"], W["w_mem_kv"], (2, 3))
        outs = emit_phase_b(k, W, x2s, kT_all, v_all, km_all, kT_loc, v_loc, blkind, tri_in, bvalid, y, nsup, x2w,
                            gath + kvw)
        P.emit(final_waits=outs[-NDMA_SEM:])
    nc.used_inputs = list(W.keys())
    return nc


def phase_b_consts(c):
    b, h, gl = core_layout(c)
    bf = ml_dtypes.bfloat16
    valid = np.zeros((NLOC, NBLK), np.float32)
    for lb, g in enumerate(gl):
        for r in range(2):
            for i in range(NLOC):
                if glob_block(i, r) < g:
                    valid[lb, r * 16 + i] = 1.0
    blkind = np.zeros((2, 32, 4096), np.float32)
    for r in range(2):
        for i in range(NLOC):
            blkind[r, r * 16 + i, i * BLK:(i + 1) * BLK] = 1.0
    return {"blkind": blkind.astype(bf), "tri": np.triu(np.ones((128, 128), np.float32)), "bvalid": valid.reshape(1, -1)}


def phase_b_inputs(inp, c, resA):
    b, h, gl = core_layout(c)
    r0, r1 = resA[2 * b], resA[2 * b + 1]
    pos = inp["positions"][b]
    pos_loc = np.concatenate([pos[g * BLK:(g + 1) * BLK] for g in gl]).astype(np.int32)
    d = {
        "x2": np.asarray(resA[c]["x2"]),
        "kT_all": np.concatenate([np.asarray(r0["kT"]), np.asarray(r1["kT"])], axis=0),
        "v_all": np.concatenate([np.asarray(r0["v"]), np.asarray(r1["v"])], axis=0),
        "km_all": np.concatenate([np.asarray(r0["kmT"]), np.asarray(r1["kmT"])], axis=0),
        "kT_loc": np.asarray(resA[c]["kT"]),
        "v_loc": np.asarray(resA[c]["v"]),
        "pos_t": np.ascontiguousarray(pos_loc.reshape(32, 128).T),
        "mem": np.ascontiguousarray(inp["mem"][b]),
    }
    d.update(phase_b_consts(c))
    return d


_CACHE = {}


FUSED = False


def kernel(**inp):
    inp = {k2: np.asarray(v2) for k2, v2 in inp.items()}
    consts = host_consts()
    out = np.zeros((NBATCH, SEQ, D), np.float32)
    if FUSED:
        if "f" not in _CACHE:
            _CACHE["f"] = build_fused()
        ncF = _CACHE["f"]
        maps = []
        keep = set(ncF.used_inputs) | {"x_main", "x_halo", "hflag", "blkind", "tri", "bvalid"}
        for c in range(8):
            d = phase_a_inputs(inp, c)
            d.update(consts)
            d.update(phase_b_consts(c))
            d = {k2: v2 for k2, v2 in d.items() if k2 in keep}
            d.update(weight_map(inp, keep))
            maps.append(d)
        res = run_bass_kernel_spmd(ncF, maps, core_ids=list(range(8))).results
    else:
        if "a" not in _CACHE:
            _CACHE["a"] = build_phase_a()
            _CACHE["b"] = build_phase_b()
        ncA, ncB = _CACHE["a"], _CACHE["b"]
        mapsA = []
        for c in range(8):
            d = phase_a_inputs(inp, c)
            d.update(consts)
            keep = set(ncA.used_inputs) | {"x_main", "x_halo", "hflag"}
            d = {k2: v2 for k2, v2 in d.items() if k2 in keep}
            d.update(weight_map(inp, keep))
            mapsA.append(d)
        resA = run_bass_kernel_spmd(ncA, mapsA, core_ids=list(range(8))).results
        mapsB = []
        for c in range(8):
            d = phase_b_inputs(inp, c, resA)
            d.update(consts)
            keep = set(ncB.used_inputs) | {"x2", "kT_all", "v_all", "km_all", "kT_loc", "v_loc", "blkind", "tri", "bvalid"}
            d = {k2: v2 for k2, v2 in d.items() if k2 in keep}
            d.update(weight_map(inp, keep))
            mapsB.append(d)
        res = run_bass_kernel_spmd(ncB, mapsB, core_ids=list(range(8))).results
    for c in range(8):
        b, h, gl = core_layout(c)
        yv = np.asarray(res[c]["y"])
        for i, g in enumerate(gl):
            out[b, g * BLK:(g + 1) * BLK] = yv[i * BLK:(i + 1) * BLK]
    return out
```
